# Optimizing a Trainium2 kernel written in Bass

```python
import math
import jax, jax.numpy as jnp
from jax import lax
import numpy as np

D_MODEL = 1024
BATCH = 16
SEQ = 4096
DEPTH = 4

CHUNK = 64
N_MIXERS = 2
N_MAMBA = (DEPTH + 1) // 2
N_RWKV = DEPTH // 2
N_VRES = max(N_RWKV - 1, 0)

D_FF = 2816
FFN_HALF = 0.5
NORM_EPS = 1e-6
N_NORMS = 6

M_EXPAND = 2
M_D_INNER = M_EXPAND * D_MODEL
M_HEADDIM = 64
M_HEADS = M_D_INNER // M_HEADDIM
M_GROUPS = 8
M_HPG = M_HEADS // M_GROUPS
M_D_STATE = 128
M_D_CONV = 4
M_CONV_DIM = M_D_INNER + 2 * M_GROUPS * M_D_STATE
M_D_IN_PROJ = M_D_INNER + M_CONV_DIM + M_HEADS
M_DT_MIN = 0.001
M_DT_MAX = 0.1
M_NORM_EPS = 1e-5

R_HEAD = 64
R_HEADS = D_MODEL // R_HEAD
R_DECAY_LORA = 64
R_AAA_LORA = 64
R_MV_LORA = 32
R_GATE_LORA = 128
R_N_SHIFT = 6
R_LNX_EPS = 64e-5
R_L2_EPS = 1e-12

kernel_name = "hybrid_mamba2_rwkv7_macaron_sandwich"


def rmsnorm(x, g, eps=NORM_EPS):
    xf = x.astype(jnp.float32)
    y = xf * lax.rsqrt(jnp.mean(xf * xf, axis=-1, keepdims=True) + eps)
    return (y * g.astype(jnp.float32)).astype(x.dtype)


def swiglu_ffn(x, w_in, w_out):
    gate, up = jnp.split(x @ w_in, 2, axis=-1)
    return (jax.nn.silu(gate) * up) @ w_out


def causal_depthwise_conv(x, w, b):
    k = w.shape[0]
    out = lax.conv_general_dilated(
        x, w[:, None, :].astype(x.dtype), window_strides=(1,), padding=[(k - 1, 0)],
        dimension_numbers=('NWC', 'WIO', 'NWC'), feature_group_count=x.shape[-1])
    return out + b.astype(x.dtype)


def ssd_chunked_scan(xh, dt, A, Bm, Cm):
    b, l, g, e, p = xh.shape
    n = Bm.shape[-1]
    nc = l // CHUNK

    def to_chunks(t):
        t = t.astype(jnp.float32).reshape((b, nc, CHUNK) + t.shape[2:])
        return jnp.moveaxis(t, 1, 0)

    xs = (to_chunks(xh), to_chunks(dt), to_chunks(Bm), to_chunks(Cm))
    A = A.astype(jnp.float32)
    causal = jnp.tril(jnp.ones((CHUNK, CHUNK), dtype=bool))[None, :, :, None, None]

    def step(S, inp):
        x_c, dt_c, B_c, C_c = inp
        a_cum = jnp.cumsum(dt_c * A, axis=1)
        seg = a_cum[:, :, None] - a_cum[:, None, :]
        decay = jnp.exp(jnp.where(causal, seg, -jnp.inf))
        cb = jnp.einsum('bign,bjgn->bijg', C_c, B_c)
        w_ij = cb[..., None] * decay * dt_c[:, None]
        y = jnp.einsum('bijge,bjgep->bigep', w_ij, x_c)
        y = y + jnp.einsum('bign,bgepn->bigep', C_c, S) * jnp.exp(a_cum)[..., None]
        to_end = jnp.exp(a_cum[:, -1:] - a_cum) * dt_c
        S = S * jnp.exp(a_cum[:, -1])[..., None, None] + jnp.einsum(
            'blge,blgn,blgep->bgepn', to_end, B_c, x_c)
        return S, y

    S0 = jnp.zeros((b, g, e, p, n), jnp.float32)
    _, ys = lax.scan(step, S0, xs)
    return jnp.moveaxis(ys, 0, 1).reshape(b, l, g, e, p)


def mamba2_mixer(u, in_proj, conv_w, conv_b, dt_bias, A_log, D_skip, norm_w, out_proj):
    f32 = jnp.float32
    b, l, _ = u.shape
    z, xbc, dt = jnp.split(u @ in_proj, [M_D_INNER, M_D_INNER + M_CONV_DIM], axis=-1)
    xbc = jax.nn.silu(causal_depthwise_conv(xbc, conv_w, conv_b))
    xs, Bm, Cm = jnp.split(xbc, [M_D_INNER, M_D_INNER + M_GROUPS * M_D_STATE], axis=-1)
    xh = xs.reshape(b, l, M_GROUPS, M_HPG, M_HEADDIM)
    Bm = Bm.reshape(b, l, M_GROUPS, M_D_STATE)
    Cm = Cm.reshape(b, l, M_GROUPS, M_D_STATE)
    dt = jax.nn.softplus(dt.astype(f32) + dt_bias.astype(f32)).reshape(b, l, M_GROUPS, M_HPG)
    A = -jnp.exp(A_log.astype(f32)).reshape(M_GROUPS, M_HPG)
    y = ssd_chunked_scan(xh, dt, A, Bm, Cm)
    y = y + xh.astype(f32) * D_skip.astype(f32).reshape(M_GROUPS, M_HPG, 1)
    y = y.reshape(b, l, M_D_INNER) * jax.nn.silu(z.astype(f32))
    yg = y.reshape(b, l, M_GROUPS, M_D_INNER // M_GROUPS)
    yg = yg * lax.rsqrt(jnp.mean(yg * yg, axis=-1, keepdims=True) + M_NORM_EPS)
    y = yg.reshape(b, l, M_D_INNER) * norm_w.astype(f32)
    return y.astype(u.dtype) @ out_proj


def rwkv7_recurrence(r, w, k, v, kk, alpha):
    b, l, h, n = r.shape

    def step(S, inp):
        r_t, w_t, k_t, v_t, kk_t, a_t = inp
        sa = jnp.einsum('bhvk,bhk->bhv', S, -kk_t)
        S = (S * w_t[:, :, None, :] + sa[..., None] * (kk_t * a_t)[:, :, None, :]
             + v_t[..., None] * k_t[:, :, None, :])
        return S, jnp.einsum('bhvk,bhk->bhv', S, r_t)

    xs = tuple(jnp.moveaxis(t, 1, 0) for t in (r, w, k, v, kk, alpha))
    S0 = jnp.zeros((b, h, n, n), jnp.float32)
    _, ys = lax.scan(step, S0, xs)
    return jnp.moveaxis(ys, 0, 1)


def rwkv7_mixer(u, v_first, mix, w_rkv, w_o, w0, w1, w2, a0, a1, a2, g1, g2,
                k_k, k_a, r_k, lnx_w, lnx_b, vres):
    f32 = jnp.float32
    b, l, d = u.shape
    delta = jnp.pad(u, ((0, 0), (1, 0), (0, 0)))[:, :-1] - u
    shift = lambda idx: u + delta * mix[idx]
    x_v = shift(3)
    r = shift(0) @ w_rkv[0]
    k = shift(2) @ w_rkv[1]
    v = x_v @ w_rkv[2]
    w_log = -jax.nn.softplus(-(w0 + jnp.tanh(shift(1) @ w1) @ w2).astype(f32)) - 0.5
    decay = jnp.exp(-jnp.exp(w_log))
    if vres is not None:
        v0, v1, v2 = vres
        v = v + (v_first - v) * jax.nn.sigmoid(v0 + (x_v @ v1) @ v2)
    alpha = jax.nn.sigmoid((a0 + (shift(4) @ a1) @ a2).astype(f32))
    g = jax.nn.sigmoid(shift(5) @ g1) @ g2

    heads = lambda t: t.astype(f32).reshape(b, l, R_HEADS, R_HEAD)
    kk = heads(k * k_k)
    kk = kk / jnp.maximum(jnp.sqrt(jnp.sum(kk * kk, axis=-1, keepdims=True)), R_L2_EPS)
    alpha_h = heads(alpha)
    k_h = heads(k) * (1.0 + (alpha_h - 1.0) * k_a.astype(f32).reshape(R_HEADS, R_HEAD))
    r_h, v_h = heads(r), heads(v)
    y = rwkv7_recurrence(r_h, heads(decay), k_h, v_h, kk, alpha_h)
    mu = jnp.mean(y, axis=-1, keepdims=True)
    var = jnp.mean(jnp.square(y - mu), axis=-1, keepdims=True)
    y = ((y - mu) * lax.rsqrt(var + R_LNX_EPS) * lnx_w.astype(f32).reshape(R_HEADS, R_HEAD)
         + lnx_b.astype(f32).reshape(R_HEADS, R_HEAD))
    y = y + jnp.sum(r_h * k_h * r_k.astype(f32), axis=-1, keepdims=True) * v_h
    y = y.reshape(b, l, d).astype(u.dtype)
    return (y * g) @ w_o, v


def setup_inputs(seed: int = 0) -> dict:
    key = jax.random.key(seed)
    ks = iter(jax.random.split(key, 40))
    nrm = lambda shape, scale: scale * jax.random.normal(next(ks), shape, jnp.float32)
    uni = lambda shape, lo, hi: jax.random.uniform(next(ks), shape, jnp.float32, lo, hi)
    D = D_MODEL
    x = nrm((BATCH, SEQ, D), 1.0)
    norms = 1.0 + nrm((DEPTH, N_NORMS, D), 0.02)
    ffn_w_in = nrm((DEPTH, 2, D, 2 * D_FF), D ** -0.5)
    ffn_w_out = nrm((DEPTH, 2, D_FF, D), D_FF ** -0.5)
    m_in_proj = nrm((N_MAMBA, D, M_D_IN_PROJ), D ** -0.5)
    m_conv_w = nrm((N_MAMBA, M_D_CONV, M_CONV_DIM), M_D_CONV ** -0.5)
    m_conv_b = nrm((N_MAMBA, M_CONV_DIM), 0.02)
    dt0 = jnp.maximum(jnp.exp(uni((N_MAMBA, M_HEADS), math.log(M_DT_MIN), math.log(M_DT_MAX))), 1e-4)
    m_dt_bias = dt0 + jnp.log(-jnp.expm1(-dt0))
    m_A_log = jnp.log(uni((N_MAMBA, M_HEADS), 1.0, 16.0))
    m_D = 1.0 + nrm((N_MAMBA, M_HEADS), 0.1)
    m_norm_w = 1.0 + nrm((N_MAMBA, M_D_INNER), 0.02)
    m_out_proj = nrm((N_MAMBA, M_D_INNER, D), M_D_INNER ** -0.5)
    r_mix = uni((N_RWKV, R_N_SHIFT, D), 0.0, 1.0)
    r_w_rkv = nrm((N_RWKV, 3, D, D), D ** -0.5)
    r_w_o = nrm((N_RWKV, D, D), D ** -0.5)
    r_w0 = uni((N_RWKV, D), -6.0, 0.0)
    r_w1 = nrm((N_RWKV, D, R_DECAY_LORA), D ** -0.5)
    r_w2 = nrm((N_RWKV, R_DECAY_LORA, D), 0.1 * R_DECAY_LORA ** -0.5)
    r_a0 = nrm((N_RWKV, D), 0.1)
    r_a1 = nrm((N_RWKV, D, R_AAA_LORA), D ** -0.5)
    r_a2 = nrm((N_RWKV, R_AAA_LORA, D), 0.1 * R_AAA_LORA ** -0.5)
    r_v0 = 1.0 + nrm((N_VRES, D), 0.1)
    r_v1 = nrm((N_VRES, D, R_MV_LORA), D ** -0.5)
    r_v2 = nrm((N_VRES, R_MV_LORA, D), 0.1 * R_MV_LORA ** -0.5)
    r_g1 = nrm((N_RWKV, D, R_GATE_LORA), D ** -0.5)
    r_g2 = nrm((N_RWKV, R_GATE_LORA, D), R_GATE_LORA ** -0.5)
    r_k_k = 0.85 + nrm((N_RWKV, D), 0.02)
    r_k_a = 1.0 + nrm((N_RWKV, D), 0.02)
    r_r_k = nrm((N_RWKV, R_HEADS, R_HEAD), 0.1)
    r_lnx_w = 1.0 + nrm((N_RWKV, D), 0.02)
    r_lnx_b = nrm((N_RWKV, D), 0.02)
    return {"x": x, "norms": norms, "ffn_w_in": ffn_w_in, "ffn_w_out": ffn_w_out,
            "m_in_proj": m_in_proj, "m_conv_w": m_conv_w, "m_conv_b": m_conv_b,
            "m_dt_bias": m_dt_bias, "m_A_log": m_A_log, "m_D": m_D, "m_norm_w": m_norm_w,
            "m_out_proj": m_out_proj,
            "r_mix": r_mix, "r_w_rkv": r_w_rkv, "r_w_o": r_w_o, "r_w0": r_w0, "r_w1": r_w1,
            "r_w2": r_w2, "r_a0": r_a0, "r_a1": r_a1, "r_a2": r_a2, "r_v0": r_v0, "r_v1": r_v1,
            "r_v2": r_v2, "r_g1": r_g1, "r_g2": r_g2, "r_k_k": r_k_k, "r_k_a": r_k_a,
            "r_r_k": r_r_k, "r_lnx_w": r_lnx_w, "r_lnx_b": r_lnx_b}


def reference(x, norms, ffn_w_in, ffn_w_out,
              m_in_proj, m_conv_w, m_conv_b, m_dt_bias, m_A_log, m_D, m_norm_w, m_out_proj,
              r_mix, r_w_rkv, r_w_o, r_w0, r_w1, r_w2, r_a0, r_a1, r_a2, r_v0, r_v1, r_v2,
              r_g1, r_g2, r_k_k, r_k_a, r_r_k, r_lnx_w, r_lnx_b):
    h = x
    v_first = None
    for i in range(DEPTH):
        t = swiglu_ffn(rmsnorm(h, norms[i, 0]), ffn_w_in[i, 0], ffn_w_out[i, 0])
        h = h + FFN_HALF * rmsnorm(t, norms[i, 1])
        u = rmsnorm(h, norms[i, 2])
        j = i // N_MIXERS
        if i % N_MIXERS == 0:
            t = mamba2_mixer(u, m_in_proj[j], m_conv_w[j], m_conv_b[j], m_dt_bias[j],
                             m_A_log[j], m_D[j], m_norm_w[j], m_out_proj[j])
        else:
            vres = (r_v0[j - 1], r_v1[j - 1], r_v2[j - 1]) if j > 0 else None
            t, v_layer = rwkv7_mixer(u, v_first, r_mix[j], r_w_rkv[j], r_w_o[j], r_w0[j], r_w1[j],
                                     r_w2[j], r_a0[j], r_a1[j], r_a2[j], r_g1[j], r_g2[j],
                                     r_k_k[j], r_k_a[j], r_r_k[j], r_lnx_w[j], r_lnx_b[j], vres)
            if j == 0:
                v_first = v_layer
        h = h + rmsnorm(t, norms[i, 3])
        t = swiglu_ffn(rmsnorm(h, norms[i, 4]), ffn_w_in[i, 1], ffn_w_out[i, 1])
        h = h + FFN_HALF * rmsnorm(t, norms[i, 5])
    return h
```

```python
import numpy as np
from contextlib import ExitStack
import concourse.bass as bass
import concourse.mybir as mybir
from concourse.bass_utils import run_bass_kernel_spmd

F32 = mybir.dt.float32
F32R = mybir.dt.float32r
ALU = mybir.AluOpType
AF = mybir.ActivationFunctionType
AX = mybir.AxisListType

D = 1024
DFF = 2816
T = 512
TMM = 256
TMR = 128
CH = 64
UW = 20480
NSLOT = 2
SLOTW = 4096
SAME_ENG_SYNC = 1


def fm(v, n=8):
    return np.ascontiguousarray(np.asarray(v, np.float32).reshape(n, 128).T)


class ColPack:
    def __init__(self):
        self.cols = []
        self.off = {}
        self.n = 0

    def add(self, name, arr):
        arr = np.asarray(arr, np.float32)
        assert arr.shape[0] == 128
        arr = arr.reshape(128, -1)
        self.off[name] = self.n
        self.n += arr.shape[1]
        self.cols.append(arr)

    def get(self):
        return np.ascontiguousarray(np.concatenate(self.cols, axis=1))


def pack_consts(inp, layers):
    cp = ColPack()
    for li in layers:
        for k in range(6):
            cp.add(f"n{li}_{k}", fm(inp["norms"][li, k]))
        j = li // 2
        if li % 2 == 0:
            cw = np.asarray(inp["m_conv_w"][j], np.float32)
            cp.add(f"cw{j}", np.ascontiguousarray(cw.reshape(4, 32, 128).transpose(2, 1, 0)))
            cp.add(f"cb{j}", fm(inp["m_conv_b"][j], 32))
            cp.add(f"mnw{j}", fm(inp["m_norm_w"][j], 16))
            cp.add(f"mD{j}", fm(np.repeat(np.asarray(inp["m_D"][j], np.float32), 64), 16))
            cp.add(f"dtb{j}", np.broadcast_to(np.asarray(inp["m_dt_bias"][j], np.float32)[None, :], (128, 32)))
            cp.add(f"alog{j}", np.broadcast_to(np.asarray(inp["m_A_log"][j], np.float32)[None, :], (128, 32)))
        else:
            for k in range(6):
                cp.add(f"mix{j}_{k}", fm(inp["r_mix"][j, k]))
            for nm in ("r_w0", "r_a0", "r_k_k", "r_k_a", "r_lnx_w", "r_lnx_b"):
                cp.add(f"{nm}{j}", fm(inp[nm][j]))
            cp.add(f"r_r_k{j}", fm(np.asarray(inp["r_r_k"][j], np.float32).reshape(-1)))
            if j > 0:
                cp.add(f"r_v0{j}", fm(inp["r_v0"][j - 1]))
    return cp


def const_mats():
    kc = np.zeros((128, 704), np.float32)
    kc[:, 0:128] = np.eye(128)
    kc[:, 128:256] = 1.0
    kc[:, 256:320] = -1.0
    s = np.arange(128) % 64
    t = np.arange(64)
    kc[:, 320:384] = (s[:, None] <= t[None, :])
    kc[:, 384:448] = (s[:, None] < t[None, :])
    kc[:, 448:512] = (s[:, None] > t[None, :])
    kc[:, 512:576] = (s[:, None] == t[None, :])
    blk = (np.arange(128)[:, None] // 64) == (np.arange(128)[None, :] // 64)
    kc[:, 576:704] = blk
    return kc


def tile_cols(w, blk, per_slot):
    K, N = w.shape
    ko = K // 128
    nb = N // (blk * per_slot)
    a = w.reshape(ko, 128, nb, per_slot, blk).transpose(2, 1, 3, 0, 4)
    return np.ascontiguousarray(a).reshape(nb, 128, per_slot * ko * blk)


def pack_weights(inp, layers):
    W = {}
    for li in layers:
        j = li // 2
        for w in range(2):
            win = np.asarray(inp["ffn_w_in"][li, w], np.float32)
            g = tile_cols(win[:, :DFF], 256, 1).reshape(11, 128, 1, 2048)
            u = tile_cols(win[:, DFF:], 256, 1).reshape(11, 128, 1, 2048)
            W[f"wi{li}_{w}"] = np.ascontiguousarray(np.concatenate([g, u], axis=2)).reshape(11, 128, 4096)
            W[f"wo{li}_{w}"] = tile_cols(np.asarray(inp["ffn_w_out"][li, w], np.float32), 128, 1)
        if li % 2 == 0:
            ip = np.asarray(inp["m_in_proj"][j], np.float32)
            W[f"mi{j}"] = tile_cols(ip[:, :6144], 256, 2)
            W[f"md{j}"] = tile_cols(ip[:, 6144:6176], 32, 1)[0]
            W[f"mo{j}"] = tile_cols(np.asarray(inp["m_out_proj"][j], np.float32), 128, 2)
        else:
            mats = [inp["r_w_rkv"][j, 0], inp["r_w_rkv"][j, 1], inp["r_w_rkv"][j, 2], inp["r_w_o"][j]]
            W[f"rw{j}"] = np.stack([tile_cols(np.asarray(m, np.float32), 128, 4) for m in mats])
            W[f"rl{j}_w1"] = tile_cols(np.asarray(inp["r_w1"][j], np.float32), 64, 1)[0]
            W[f"rl{j}_w2"] = np.ascontiguousarray(np.asarray(inp["r_w2"][j], np.float32))
            W[f"rl{j}_a1"] = tile_cols(np.asarray(inp["r_a1"][j], np.float32), 64, 1)[0]
            W[f"rl{j}_a2"] = np.ascontiguousarray(np.asarray(inp["r_a2"][j], np.float32))
            W[f"rl{j}_g1"] = tile_cols(np.asarray(inp["r_g1"][j], np.float32), 128, 1)[0]
            W[f"rl{j}_g2"] = np.ascontiguousarray(np.asarray(inp["r_g2"][j], np.float32))
            if j > 0:
                W[f"rl{j}_v1"] = tile_cols(np.asarray(inp["r_v1"][j - 1], np.float32), 32, 1)[0]
                W[f"rl{j}_v2"] = np.ascontiguousarray(np.asarray(inp["r_v2"][j - 1], np.float32))
    return W


class Sched:
    def __init__(self, nc, stack):
        self.nc = nc
        self.stack = stack
        self.eng = dict(pe=nc.tensor, act=nc.scalar, dve=nc.vector, pool=nc.gpsimd, sp=nc.sync)
        self.sem = {}
        self.cnt = {}
        for e in self.eng:
            self.sem[e] = stack.enter_context(nc.semaphore("s_" + e))
            self.cnt[e] = 0
        self.seen = {e: {} for e in self.eng}
        self.res = {}
        self.dsem = {}
        self.dcnt = {}
        self.psp = 0
        self.wcnt = 0

    def _deps(self, r, w):
        deps = []
        for k in r:
            st = self.res.get(k)
            if st and st[0]:
                deps.append(st[0])
        for k in w:
            st = self.res.get(k)
            if st:
                if st[0]:
                    deps.append(st[0])
                deps.extend(st[1].values())
        return deps

    def _wait(self, e, deps):
        best = {}
        for (sname, sem, val) in deps:
            if sname == e and (e == "pe" or not SAME_ENG_SYNC):
                continue
            if best.get(sname, (None, 0))[1] < val:
                best[sname] = (sem, val)
        for sname, (sem, val) in best.items():
            if self.seen[e].get(sname, 0) >= val:
                continue
            self.eng[e].wait_ge(sem, val)
            self.seen[e][sname] = val

    def _record(self, tok, r, w):
        for k in r:
            st = self.res.setdefault(k, [None, {}])
            st[1][tok[0]] = tok
        for k in w:
            self.res[k] = [tok, {}]

    def op(self, e, fn, r=(), w=()):
        self._wait(e, self._deps(r, w))
        inst = fn(self.eng[e])
        self.cnt[e] += 1
        inst.then_inc(self.sem[e], 1)
        self._record((e, self.sem[e], self.cnt[e]), r, w)
        return inst

    def dma(self, q, out, in_, r=(), w=(), stream="d0"):
        if stream not in self.dsem:
            self.dsem[stream] = self.stack.enter_context(self.nc.semaphore("d_" + stream))
            self.dcnt[stream] = 0
        self._wait(q, self._deps(r, w))
        inst = self.eng[q].dma_start(out=out, in_=in_)
        self.dcnt[stream] += 16
        inst.then_inc(self.dsem[stream], 16)
        self._record(("D" + stream, self.dsem[stream], self.dcnt[stream]), r, w)

    def wait_keys(self, e, keys):
        deps = []
        for k in keys:
            st = self.res.get(k)
            if st:
                if st[0]:
                    deps.append(st[0])
                deps.extend(st[1].values())
        self._wait(e, deps)

    def barrier(self):
        for e in ("pe", "act", "dve", "pool"):
            for o in ("pe", "act", "dve", "pool"):
                if o == e or self.cnt[o] == 0:
                    continue
                if self.seen[e].get(o, 0) >= self.cnt[o]:
                    continue
                self.eng[e].wait_ge(self.sem[o], self.cnt[o])
                self.seen[e][o] = self.cnt[o]

    def mm(self, out, lhsT, rhs, start, stop, r, w):
        if not (lhsT.dtype == F32R and rhs.dtype == F32R):
            lhsT, rhs = rd(lhsT), rd(rhs)
        return self.op("pe", lambda e: e.matmul(out, lhsT=lhsT, rhs=rhs, start=start, stop=stop, skip_group_check=True), r=r, w=w)

    def act(self, out, in_, func, r, w, bias=None, scale=None):
        kw = {}
        if bias is not None:
            kw["bias"] = bias
        if scale is not None:
            kw["scale"] = scale
        out, in_ = wr(out), rd(in_)
        return self.op("act", lambda e: e.activation(out=out, in_=in_, func=func, **kw), r=r, w=w)

    def tt(self, out, in0, in1, op, r, w, eng="dve"):
        out, in0, in1 = wr(out), rd(in0), rd(in1)
        return self.op(eng, lambda e: e.tensor_tensor(out=out, in0=in0, in1=in1, op=op), r=r, w=w)

    def ts(self, out, in0, s1, op0, r, w, s2=None, op1=None, eng="dve"):
        out, in0 = wr(out), rd(in0)
        if op1 is None:
            return self.op(eng, lambda e: e.tensor_scalar(out=out, in0=in0, scalar1=s1, scalar2=None, op0=op0), r=r, w=w)
        return self.op(eng, lambda e: e.tensor_scalar(out=out, in0=in0, scalar1=s1, scalar2=s2, op0=op0, op1=op1), r=r, w=w)

    def stt(self, out, in0, scalar, in1, op0, op1, r, w):
        out, in0, in1 = wr(out), rd(in0), rd(in1)
        return self.op("dve", lambda e: e.scalar_tensor_tensor(out=out, in0=in0, scalar=scalar, in1=in1, op0=op0, op1=op1), r=r, w=w)

    def cp(self, out, in_, r, w, eng="dve"):
        out, in_ = wr(out), rd(in_)
        return self.op(eng, lambda e: e.tensor_copy(out=out, in_=in_), r=r, w=w)


def bc(ap, shape):
    return ap.to_broadcast(list(shape))


RNAMES = ("U", "xn")


def rd(ap):
    if hasattr(ap, "dtype") and ap.dtype == F32R:
        return ap.bitcast(F32)
    return ap


def wr(ap):
    if ap.dtype == F32 and ap.tensor.name in RNAMES:
        return ap.bitcast(F32R)
    return ap


DEBUG = []


NST = 2 * 2048 + 2 * 96 + 2 * 512 + 2 * 8


def build(NSEQ, L, layers, cpoff, ncp, wshapes):
    nc = bass.Bass("TRN2", target_bir_lowering=False)
    nc.dge_precook = False
    xT = nc.dram_tensor("xT", [NSEQ, 128, 8, L], F32, kind="ExternalInput").ap()
    outT = nc.dram_tensor("outT", [NSEQ, 128, 8, L], F32, kind="ExternalOutput").ap()
    st_in = nc.dram_tensor("st_in", [NSEQ, 128, NST], F32, kind="ExternalInput").ap()
    st_out = nc.dram_tensor("st_out", [NSEQ, 128, NST], F32, kind="ExternalOutput").ap()
    cpd = nc.dram_tensor("cp", [128, ncp], F32, kind="ExternalInput").ap()
    kcd = nc.dram_tensor("kc", [128, 704], F32, kind="ExternalInput").ap()
    onesd = nc.dram_tensor("ones_r", [128, 128], F32R, kind="ExternalInput").ap()
    Wd = {}
    for name, shp in wshapes.items():
        Wd[name] = nc.dram_tensor(name, list(shp), F32R, kind="ExternalInput").ap()

    with ExitStack() as st:
        st.enter_context(nc.allow_low_precision(reason="float32r (11-bit mantissa) storage for matmul operands"))
        S = Sched(nc, st)
        sb = lambda name, shape, dt=F32: st.enter_context(nc.sbuf_tensor(name, shape, dt))
        h = sb("h", [128, 8, T])
        xn_t = sb("xn", [128, 8, T], F32R)
        xn_r = xn_t[:]
        xn = xn_r.bitcast(F32)
        vf = sb("vf", [128, 8, T])
        U_t = sb("U", [128, UW], F32R)
        U_r = U_t[:]
        U = U_r.bitcast(F32)
        ring = sb("ring", [128, NSLOT * SLOTW], F32R)
        cpt = sb("cpt", [128, ncp])
        kc = sb("kct", [128, 704])
        ones_r = sb("ones_rt", [128, 128], F32R)
        rstd = sb("rstd", [128, T])
        rstd2 = sb("rstd2", [128, 8, TMM])
        sg = sb("sg", [128, 2, T])
        nm = len([l for l in layers if l % 2 == 0])
        nr = len([l for l in layers if l % 2 == 1])
        Sm = {l // 2: sb(f"Sm{l}", [128, 32 * 64]) for l in layers if l % 2 == 0}
        Cc = {l // 2: sb(f"Cc{l}", [128, 32, 3]) for l in layers if l % 2 == 0}
        Arep = {l // 2: sb(f"Ar{l}", [128, 32]) for l in layers if l % 2 == 0}
        Sr = {l // 2: sb(f"Sr{l}", [128, 8, 64]) for l in layers if l % 2 == 1}
        Up = {l // 2: sb(f"Up{l}", [128, 8, 1]) for l in layers if l % 2 == 1}
        sm = sb("small", [128, 1024])
        PS = st.enter_context(nc.psum_tensor("PS", [128, 4096], F32))

        ident = kc[:, 0:128]
        ones = kc[:, 128:256]
        negones = kc[:, 256:320]
        tri_le = kc[:, 320:384]
        tri_lt = kc[:, 384:448]
        tri_gt = kc[:, 448:512]
        i64 = kc[:, 512:576]
        blk1 = kc[:, 576:704]

        def ps(nb):
            if S.psp + nb > 8:
                S.psp = 0
            b0 = S.psp
            S.psp = (S.psp + nb) % 8
            return PS[:, b0 * 512:(b0 + nb) * 512], [f"ps{b}" for b in range(b0, b0 + nb)]

        def wload(src, npart=128, ncols=SLOTW):
            slot = S.wcnt % NSLOT
            S.wcnt += 1
            key = f"w{slot}"
            dst = ring[0:npart, slot * SLOTW: slot * SLOTW + ncols]
            S.dma("sp", dst, src, w=[key], stream=key)
            return dst, key

        dbg_n = [0]

        def dbg(name, ap, npart=128):
            if not DEBUG or dbg_n[0] >= 40:
                return
            dbg_n[0] += 1
            ap = rd(ap)
            shp = list(ap.shape)
            d = nc.dram_tensor("dbg_" + name, shp, F32, kind="ExternalOutput").ap()
            S.barrier()
            for e in ("pe", "act", "dve"):
                if S.cnt[e] and S.seen["pool"].get(e, 0) < S.cnt[e]:
                    S.eng["pool"].wait_ge(S.sem[e], S.cnt[e])
                    S.seen["pool"][e] = S.cnt[e]
            S.dma("pool", d, ap, w=["DBG" + name], stream="dbg")
            for e in ("pool", "pe", "act", "dve"):
                S.wait_keys(e, ["DBG" + name])

        def cpc(name, n=8):
            o = cpoff[name]
            return cpt[:, o:o + n]

        S.dma("pool", cpt[:], cpd, w=["cpt"], stream="c0")
        S.dma("pool", kc[:], kcd, w=["kc"], stream="c1")
        S.dma("pool", ones_r[:], onesd, w=["ones_r"], stream="c2")
        CK = ["cpt", "kc", "ones_r"]
        for l in layers:
            if l % 2 == 0:
                j = l // 2
                o = cpoff[f"alog{j}"]
                S.act(Arep[j][:], cpt[:, o:o + 32], AF.Exp, r=CK, w=[f"Ar{j}"])
                S.ts(Arep[j][:], Arep[j][:], -1.0, ALU.mult, r=[f"Ar{j}"], w=[f"Ar{j}"])

        def sumsq_rstd(src, nck, n, div, eps, rkeys, sqv, out_rstd, okey):
            S.act(sqv, src, AF.Square, r=rkeys, w=["sq"])
            p, pk = ps(1)
            for c in range(nck):
                S.mm(p[:, 0:n], ones_r[:], sqv[:, c, :], c == 0, c == nck - 1, r=["sq", "ones_r"], w=pk)
            S.ts(out_rstd, p[:, 0:n], 1.0 / div, ALU.mult, r=pk, w=[okey], s2=eps, op1=ALU.add)
            S.act(out_rstd, out_rstd, AF.Sqrt, r=[okey], w=[okey])
            S.op("dve", lambda e: e.reciprocal(out=out_rstd, in_=out_rstd), r=[okey], w=[okey])

        def prenorm(gname, n0, n):
            hv = h[:, :, n0:n0 + n]
            sqv = U_r[:, 16384:16384 + 8 * n].rearrange("p (c t) -> p c t", c=8)
            sumsq_rstd(hv, 8, n, 1024.0, 1e-6, ["h"], sqv, rstd[:, 0:n], "rstd")
            xv = xn_r[:, :, n0:n0 + n]
            S.tt(xv, hv, bc(rstd[:, 0:n].unsqueeze(1), [128, 8, n]), ALU.mult, r=["h", "rstd"], w=["xn"])
            S.tt(xv, xv, bc(cpc(gname).unsqueeze(2), [128, 8, n]), ALU.mult, r=["xn", "cpt"], w=["xn"])

        def postnorm_add(gname, half):
            tv = xn[:, :, :]
            sqv = U_r[:, 16384:16384 + 8 * T].rearrange("p (c t) -> p c t", c=8)
            sumsq_rstd(tv, 8, T, 1024.0, 1e-6, ["xn"], sqv, rstd[:, :], "rstd")
            S.tt(tv, tv, bc(rstd[:, :].unsqueeze(1), [128, 8, T]), ALU.mult, r=["xn", "rstd"], w=["xn"])
            S.tt(tv, tv, bc(cpc(gname).unsqueeze(2), [128, 8, T]), ALU.mult, r=["xn", "cpt"], w=["xn"])
            S.stt(h[:, :, :], tv, half, h[:, :, :], ALU.mult, ALU.add, r=["xn", "h"], w=["h"])

        def ffn(li, w):
            prenorm(f"n{li}_{0 if w == 0 else 4}", 0, T)
            hid = U_r[:, 0:22 * T].rearrange("p (j t) -> p j t", j=22)
            for b in range(11):
                wt, wk = wload(Wd[f"wi{li}_{w}"][b])
                wv = wt.rearrange("p (g k c) -> p g k c", g=2, k=8)
                for jj in range(2):
                    j = 2 * b + jj
                    pg, pgk = ps(1)
                    for ko in range(8):
                        S.mm(pg, wv[:, 0, ko, jj * 128:(jj + 1) * 128], xn_r[:, ko, :], ko == 0, ko == 7, r=[wk, "xn"], w=pgk)
                    pu, puk = ps(1)
                    for ko in range(8):
                        S.mm(pu, wv[:, 1, ko, jj * 128:(jj + 1) * 128], xn_r[:, ko, :], ko == 0, ko == 7, r=[wk, "xn"], w=puk)
                    sgk = f"sg{j % 2}"
                    S.act(sg[:, j % 2, :], pg, AF.Silu, r=pgk, w=[sgk])
                    S.tt(hid[:, j, :], sg[:, j % 2, :], pu, ALU.mult, r=[sgk] + puk, w=["hid"])
            for c in range(8):
                wt, wk = wload(Wd[f"wo{li}_{w}"][c], ncols=2816)
                wv = wt.rearrange("p (j m) -> p j m", j=22)
                p, pk = ps(1)
                for j in range(22):
                    S.mm(p, wv[:, j, :], hid[:, j, :], j == 0, j == 21, r=[wk, "hid"], w=pk)
                S.act(xn[:, c, :], p, AF.Copy, r=pk, w=["xn"])
            postnorm_add(f"n{li}_{1 if w == 0 else 5}", 0.5)

        def mamba(li, hs):
            j = li // 2
            n = TMM
            u_r = xn_r[:, :, hs:hs + n]
            u_f = xn[:, :, hs:hs + n]
            zs = U[:, 0:16 * n].rearrange("p (c t) -> p c t", c=16)
            xc = U[:, 4096:4096 + 32 * n].rearrange("p (c t) -> p c t", c=32)
            raw = U[:, 12288:12288 + 2 * 260].rearrange("p (b t) -> p b t", b=2)
            acc = U[:, 12808:12808 + 2 * 256].rearrange("p (b t) -> p b t", b=2)
            TB = 13320
            cwo = cpoff[f"cw{j}"]
            cbo = cpoff[f"cb{j}"]
            wdt, wdk = wload(Wd[f"md{j}"], ncols=256)
            wdv = wdt.bitcast(F32).rearrange("p (k c) -> p k c", k=8)
            dtr = sm[0:64, 0:128].rearrange("p (q h) -> p q h", q=4)
            for q in range(4):
                p, pk = ps(1)
                for ko in range(8):
                    S.mm(p[0:64, 0:32], u_f[:, ko, q * 64:(q + 1) * 64], wdv[:, ko, :], ko == 0, ko == 7, r=[wdk, "xn"], w=pk)
                o = cpoff[f"dtb{j}"]
                S.tt(dtr[:, q, :], p[0:64, 0:32], cpt[0:64, o:o + 32], ALU.add, r=pk + ["cpt"], w=["dtr"])
            ci = 0
            for b2 in range(12):
                wt, wk = wload(Wd[f"mi{j}"][b2])
                wv = wt.rearrange("p (s k c) -> p s k c", s=2, k=8)
                for s2 in range(2):
                    for cc in range(2):
                        fc = (2 * b2 + s2) * 2 + cc
                        p, pk = ps(1)
                        for ko in range(8):
                            S.mm(p[:, 0:n], wv[:, s2, ko, cc * 128:(cc + 1) * 128], u_r[:, ko, :], ko == 0, ko == 7, r=[wk, "xn"], w=pk)
                        if fc < 16:
                            S.act(zs[:, fc, :], p[:, 0:n], AF.Silu, r=pk, w=["zs"])
                        else:
                            c = fc - 16
                            bsel = ci % 2
                            ci += 1
                            rk, ak = f"raw{bsel}", f"acc{bsel}"
                            S.act(raw[:, bsel, 3:3 + n], p[:, 0:n], AF.Copy, r=pk, w=[rk])
                            S.cp(raw[:, bsel, 0:3], Cc[j][:, c, :], r=[f"Cc{j}"], w=[rk], eng="pool")
                            S.ts(acc[:, bsel, :], raw[:, bsel, 0:n], cpt[:, cwo + 4 * c:cwo + 4 * c + 1], ALU.mult, r=[rk, "cpt"], w=[ak])
                            for k in range(1, 4):
                                S.stt(acc[:, bsel, :], raw[:, bsel, k:k + n], cpt[:, cwo + 4 * c + k:cwo + 4 * c + k + 1], acc[:, bsel, :],
                                      ALU.mult, ALU.add, r=[rk, ak, "cpt"], w=[ak])
                            S.cp(Cc[j][:, c, :], raw[:, bsel, n:n + 3], r=[rk], w=[f"Cc{j}"], eng="pool")
                            S.act(xc[:, c, :], acc[:, bsel, :], AF.Silu, r=[ak, "cpt"], w=["xc"], bias=cpt[:, cbo + c:cbo + c + 1])
            dt = sm[0:64, 128:256].rearrange("p (q h) -> p q h", q=4)
            dtA = sm[0:64, 256:384].rearrange("p (q h) -> p q h", q=4)
            S.act(dt, dtr, AF.Exp, r=["dtr"], w=["dt"])
            S.act(dt, dt, AF.Ln, r=["dt"], w=["dt"], bias=1.0)
            S.tt(dtA, dt, bc(Arep[j][0:64, :].unsqueeze(1), [64, 4, 32]), ALU.mult, r=["dt", f"Ar{j}"], w=["dtA"])
            acum = sm[0:64, 384:416]
            eac = sm[0:64, 416:448]
            toe = sm[0:64, 448:480]
            eL = sm[:, 480:512]
            CBm = sm[0:64, 512:1024].rearrange("p (g i) -> p g i", g=8)
            Btm = U[0:64, TB:TB + 1024].rearrange("p (g n) -> p g n", g=8)
            xdt = U[0:64, TB + 1024:TB + 2048].rearrange("p (h d) -> p h d", h=16)
            Xd = U[0:64, TB + 2048:TB + 3072].rearrange("p (h d) -> p h d", h=16)
            dec = U[0:64, TB + 3072:TB + 4096].rearrange("p (h d) -> p h d", h=16)
            ys = U[0:64, TB + 4096:TB + 5120].rearrange("p (h d) -> p h d", h=16)
            xw = U[0:64, TB + 5120:TB + 6144].rearrange("p (h d) -> p h d", h=16)
            y1 = U[:, TB + 6144:TB + 6656].rearrange("p (c t) -> p c t", c=8)
            Smv = Sm[j][:, :].rearrange("p (h d) -> p h d", h=32)
            smk = f"Sm{j}"
            for q in range(4):
                cs = slice(q * 64, (q + 1) * 64)
                p, pk = ps(1)
                S.mm(p[0:64, 0:32], tri_le[0:64, :], dtA[:, q, :], True, True, r=["kc", "dtA"], w=pk)
                S.mm(p[:, 32:64], ones[0:64, :], dtA[:, q, :], True, True, r=["kc", "dtA"], w=pk)
                S.act(acum, p[0:64, 0:32], AF.Copy, r=pk, w=["acum"])
                S.act(eac, p[0:64, 0:32], AF.Exp, r=pk, w=["eac"])
                S.tt(toe, p[0:64, 32:64], acum, ALU.subtract, r=pk + ["acum"], w=["toe"])
                S.act(toe, toe, AF.Exp, r=["toe"], w=["toe"])
                S.act(eL, p[:, 32:64], AF.Exp, r=pk, w=["eL"])
                p, pk = ps(2)
                for g in range(8):
                    S.mm(p[0:64, g * 128:(g + 1) * 128], xc[:, 16 + g, cs], ident, True, True, r=["xc", "kc"], w=pk)
                S.act(Btm, p[0:64, 0:1024].rearrange("p (g n) -> p g n", g=8), AF.Copy, r=pk, w=["Btm"])
                p, pk = ps(1)
                for g in range(8):
                    S.mm(p[0:64, g * 64:(g + 1) * 64], xc[:, 16 + g, cs], xc[:, 24 + g, cs], True, True, r=["xc"], w=pk)
                S.tt(CBm, p[0:64, 0:512].rearrange("p (g i) -> p g i", g=8), bc(tri_le[0:64, :].unsqueeze(1), [64, 8, 64]), ALU.mult,
                     r=pk + ["kc"], w=["CBm"])
                for hh in range(2):
                    h0 = 16 * hh
                    p, pk = ps(2)
                    for cc in range(8):
                        S.mm(p[0:64, cc * 128:(cc + 1) * 128], xc[:, 8 * hh + cc, cs], ident, True, True, r=["xc", "kc"], w=pk)
                    S.tt(xdt, p[0:64, 0:1024].rearrange("p (h d) -> p h d", h=16), bc(dt[:, q, h0:h0 + 16].unsqueeze(2), [64, 16, 64]),
                         ALU.mult, r=pk + ["dt"], w=["xdt"])
                    S.tt(Xd, bc(tri_le[0:64, :].unsqueeze(1), [64, 16, 64]), bc(dtA[:, q, h0:h0 + 16].unsqueeze(2), [64, 16, 64]), ALU.mult,
                         r=["kc", "dtA"], w=["Xd"])
                    p, pk = ps(2)
                    for b in range(2):
                        S.mm(p[0:64, b * 512:(b + 1) * 512], ones[0:64, 0:64], Xd[:, 8 * b:8 * b + 8, :].rearrange("p h d -> p (h d)"), True, False,
                             r=["kc", "Xd"], w=pk)
                    for hq in range(16):
                        S.mm(p[0:64, hq * 64:(hq + 1) * 64], Xd[:, hq, :], negones[0:64, :], False, True, r=["kc", "Xd"], w=pk)
                    S.ts(dec, p[0:64, 0:1024].rearrange("p (h d) -> p h d", h=16), 0.0, ALU.min, r=pk, w=["dec"])
                    S.act(dec, dec, AF.Exp, r=["dec"], w=["dec"])
                    decv = dec.rearrange("p (g e) d -> p g e d", g=4)
                    S.tt(decv, decv, bc(CBm[:, 4 * hh:4 * hh + 4, :].unsqueeze(2), [64, 4, 4, 64]), ALU.mult, r=["dec", "CBm"], w=["dec"])
                    p, pk = ps(2)
                    for g in range(4):
                        S.mm(p[0:64, g * 256:(g + 1) * 256], xc[:, 24 + 4 * hh + g, cs],
                             Sm[j][:, (h0 + 4 * g) * 64:(h0 + 4 * g + 4) * 64], True, True, r=["xc", smk], w=pk)
                    S.tt(ys, p[0:64, 0:1024].rearrange("p (h d) -> p h d", h=16), bc(eac[:, h0:h0 + 16].unsqueeze(2), [64, 16, 64]), ALU.mult,
                         r=pk + ["eac"], w=["ys"])
                    p, pk = ps(2)
                    for hq in range(16):
                        S.mm(p[0:64, hq * 64:(hq + 1) * 64], dec[:, hq, :], xdt[:, hq, :], True, True, r=["dec", "xdt"], w=pk)
                    S.tt(ys, ys, p[0:64, 0:1024].rearrange("p (h d) -> p h d", h=16), ALU.add, r=pk + ["ys"], w=["ys"])
                    ysf = ys.rearrange("p h d -> p (h d)")
                    p, pk = ps(1)
                    for cc in range(8):
                        S.mm(p[:, cc * 64:(cc + 1) * 64], ysf[:, cc * 128:(cc + 1) * 128], ident[0:64, 0:64], True, True, r=["ys", "kc"], w=pk)
                    mo = cpoff[f"mD{j}"]
                    S.tt(y1, xc[:, 8 * hh:8 * hh + 8, cs], bc(cpt[:, mo + 8 * hh:mo + 8 * hh + 8].unsqueeze(2), [128, 8, 64]), ALU.mult,
                         r=["xc", "cpt"], w=["y1"], eng="pool")
                    S.tt(y1, y1, p[:, 0:512].rearrange("p (c t) -> p c t", c=8), ALU.add, r=pk + ["y1"], w=["y1"])
                    S.tt(zs[:, 8 * hh:8 * hh + 8, cs], y1, zs[:, 8 * hh:8 * hh + 8, cs], ALU.mult, r=["y1", "zs"], w=["zs"])
                    S.tt(xw, xdt, bc(toe[:, h0:h0 + 16].unsqueeze(2), [64, 16, 64]), ALU.mult, r=["xdt", "toe"], w=["xw"])
                    p, pk = ps(2)
                    for g in range(4):
                        S.mm(p[:, g * 256:(g + 1) * 256], Btm[:, 4 * hh + g, :], xw[:, 4 * g:4 * g + 4, :].rearrange("p h d -> p (h d)"), True, True,
                             r=["Btm", "xw"], w=pk)
                    S.tt(Smv[:, h0:h0 + 16, :], Smv[:, h0:h0 + 16, :], bc(eL[:, h0:h0 + 16].unsqueeze(2), [128, 16, 64]), ALU.mult,
                         r=[smk, "eL"], w=[smk])
                    S.tt(Smv[:, h0:h0 + 16, :], Smv[:, h0:h0 + 16, :], p[:, 0:1024].rearrange("p (h d) -> p h d", h=16), ALU.add,
                         r=pk + [smk], w=[smk])
            sqv = U_r[:, 4096:4096 + 16 * n].rearrange("p (c t) -> p c t", c=16)
            S.act(sqv, zs, AF.Square, r=["zs"], w=["sq"])
            for g in range(8):
                p, pk = ps(1)
                for c2 in range(2):
                    S.mm(p[:, 0:n], ones_r[:], sqv[:, 2 * g + c2, :], c2 == 0, c2 == 1, r=["sq", "ones_r"], w=pk)
                S.ts(rstd2[:, g, :], p[:, 0:n], 1.0 / 256.0, ALU.mult, r=pk, w=["rstd2"], s2=1e-5, op1=ALU.add)
            S.act(rstd2[:, :, :], rstd2[:, :, :], AF.Sqrt, r=["rstd2"], w=["rstd2"])
            S.op("dve", lambda e: e.reciprocal(out=rstd2[:, :, :], in_=rstd2[:, :, :]), r=["rstd2"], w=["rstd2"])
            yn = U_r[:, 0:16 * n].rearrange("p (c t) -> p c t", c=16)
            znv = zs.rearrange("p (g e) t -> p g e t", g=8)
            S.tt(znv, znv, bc(rstd2[:, :, :].unsqueeze(2), [128, 8, 2, n]), ALU.mult, r=["zs", "rstd2"], w=["zs"])
            S.tt(yn, zs, bc(cpc(f"mnw{j}", 16).unsqueeze(2), [128, 16, n]), ALU.mult, r=["zs", "cpt"], w=["zs"])
            for c2 in range(4):
                wt, wk = wload(Wd[f"mo{j}"][c2])
                wv = wt.rearrange("p (s k m) -> p s k m", s=2, k=16)
                for s2 in range(2):
                    c = 2 * c2 + s2
                    p, pk = ps(1)
                    for k in range(16):
                        S.mm(p[:, 0:n], wv[:, s2, k, :], yn[:, k, :], k == 0, k == 15, r=[wk, "zs"], w=pk)
                    S.act(xn[:, c, hs:hs + n], p[:, 0:n], AF.Copy, r=pk, w=["xn"])

        def rwkv(li, qs):
            j = li // 2
            n = TMR
            nb = 8 * n
            u = xn[:, :, qs:qs + n]
            Bf = lambda i: U[:, i * nb:(i + 1) * nb].rearrange("p (c t) -> p c t", c=8)
            Br = lambda i: U_r[:, i * nb:(i + 1) * nb].rearrange("p (c t) -> p c t", c=8)
            dl, xm0, xm1 = Bf(0), Br(1), Br(2)
            rb, kb, vb, wl, al, gb, kk, bon, Eb, yg, tmp, tmp2 = (Bf(3), Bf(4), Bf(5), Bf(6), Bf(7), Bf(8), Bf(9), Bf(10),
                                                                  Bf(11), Br(12), Bf(13), Bf(14))
            upk = f"Up{j}"
            srk = f"Sr{j}"
            S.tt(dl[:, :, 1:n], u[:, :, 0:n - 1], u[:, :, 1:n], ALU.subtract, r=["xn"], w=["dl"])
            S.tt(dl[:, :, 0:1], Up[j][:, :, :], u[:, :, 0:1], ALU.subtract, r=["xn", upk, "dl"], w=["dl"])
            S.cp(Up[j][:, :, :], u[:, :, n - 1:n], r=["xn"], w=[upk], eng="pool")

            def mix(i, dst, dk):
                S.tt(dst, dl, bc(cpc(f"mix{j}_{i}").unsqueeze(2), [128, 8, n]), ALU.mult, r=["dl", "cpt"], w=[dk])
                S.tt(dst, dst, u, ALU.add, r=[dk, "xn"], w=[dk])

            def proj(mi, src, sk, dst, dk):
                for c4 in range(2):
                    wt, wk = wload(Wd[f"rw{j}"][mi, c4])
                    wv = wt.rearrange("p (s k m) -> p s k m", s=4, k=8)
                    p, pk = ps(1)
                    for s4 in range(4):
                        for ko in range(8):
                            S.mm(p[:, s4 * n:(s4 + 1) * n], wv[:, s4, ko, :], src[:, ko, :], ko == 0, ko == 7, r=[wk, sk], w=pk)
                    S.act(dst[:, 4 * c4:4 * c4 + 4, :], p[:, 0:4 * n].rearrange("p (c t) -> p c t", c=4), AF.Copy, r=pk, w=[dk])

            def lora(nm, r1, src, sk, midfunc, dst, dk, bias_name, outfunc):
                w1t, k1 = wload(Wd[f"rl{j}_{nm}1"], ncols=8 * r1)
                w1v = w1t.bitcast(F32).rearrange("p (k c) -> p k c", k=8)
                p, pk = ps(1)
                for ko in range(8):
                    S.mm(p[0:r1, 0:n], w1v[:, ko, :], src[:, ko, :].bitcast(F32), ko == 0, ko == 7, r=[k1, sk], w=pk)
                mid = U[0:r1, 15360:15360 + n]
                S.act(mid, p[0:r1, 0:n], midfunc, r=pk, w=["mid"])
                w2t, k2 = wload(Wd[f"rl{j}_{nm}2"], npart=r1, ncols=1024)
                w2v = w2t.bitcast(F32)
                p2, pk2 = ps(2)
                for c in range(8):
                    S.mm(p2[:, c * n:(c + 1) * n], w2v[:, c * 128:(c + 1) * 128], mid, True, True, r=[k2, "mid"], w=pk2)
                pv = p2[:, 0:8 * n].rearrange("p (c t) -> p c t", c=8)
                if bias_name is not None:
                    S.tt(dst, pv, bc(cpc(bias_name).unsqueeze(2), [128, 8, n]), ALU.add, r=pk2 + ["cpt"], w=[dk])
                    S.act(dst, dst, outfunc, r=[dk], w=[dk])
                else:
                    S.act(dst, pv, outfunc, r=pk2, w=[dk])

            mix(0, xm0, "xm0")
            proj(0, xm0, "xm0", rb, "rb")
            mix(1, xm1, "xm1")
            lora("w", 64, xm1, "xm1", AF.Tanh, wl, "wl", f"r_w0{j}", AF.Sigmoid)
            mix(2, xm0, "xm0")
            proj(1, xm0, "xm0", kb, "kb")
            mix(3, xm0, "xm0")
            proj(2, xm0, "xm0", vb, "vb")
            if j > 0:
                lora("v", 32, xm0, "xm0", AF.Copy, tmp, "tmp", f"r_v0{j}", AF.Sigmoid)
                S.tt(tmp2, vf[:, :, qs:qs + n], vb, ALU.subtract, r=["vf", "vb"], w=["tmp2"])
                S.tt(tmp2, tmp2, tmp, ALU.mult, r=["tmp2", "tmp"], w=["tmp2"])
                S.tt(vb, vb, tmp2, ALU.add, r=["vb", "tmp2"], w=["vb"])
            else:
                S.cp(vf[:, :, qs:qs + n], vb, r=["vb"], w=["vf"], eng="pool")
            mix(4, xm1, "xm1")
            lora("a", 64, xm1, "xm1", AF.Copy, al, "al", f"r_a0{j}", AF.Sigmoid)
            mix(5, xm1, "xm1")
            lora("g", 128, xm1, "xm1", AF.Sigmoid, gb, "gb", None, AF.Copy)

            S.tt(kk, kb, bc(cpc(f"r_k_k{j}").unsqueeze(2), [128, 8, n]), ALU.mult, r=["kb", "cpt"], w=["kk"])
            S.tt(tmp, kk, kk, ALU.mult, r=["kk"], w=["tmp"])
            p, pk = ps(2)
            for c in range(8):
                S.mm(p[:, c * n:(c + 1) * n], blk1, tmp[:, c, :], True, True, r=["kc", "tmp"], w=pk)
            pv = p[:, 0:8 * n].rearrange("p (c t) -> p c t", c=8)
            S.act(tmp2, pv, AF.Sqrt, r=pk, w=["tmp2"])
            S.ts(tmp2, tmp2, 1e-12, ALU.max, r=["tmp2"], w=["tmp2"])
            S.op("dve", lambda e: e.reciprocal(out=wr(tmp2), in_=tmp2), r=["tmp2"], w=["tmp2"])
            S.tt(kk, kk, tmp2, ALU.mult, r=["kk", "tmp2"], w=["kk"])
            S.ts(tmp, al, -1.0, ALU.add, r=["al"], w=["tmp"])
            S.tt(tmp, tmp, bc(cpc(f"r_k_a{j}").unsqueeze(2), [128, 8, n]), ALU.mult, r=["tmp", "cpt"], w=["tmp"])
            S.ts(tmp, tmp, 1.0, ALU.add, r=["tmp"], w=["tmp"])
            S.tt(kb, kb, tmp, ALU.mult, r=["kb", "tmp"], w=["kb"])
            S.tt(tmp, rb, kb, ALU.mult, r=["rb", "kb"], w=["tmp"])
            S.tt(tmp, tmp, bc(cpc(f"r_r_k{j}").unsqueeze(2), [128, 8, n]), ALU.mult, r=["tmp", "cpt"], w=["tmp"])
            p, pk = ps(2)
            for c in range(8):
                S.mm(p[:, c * n:(c + 1) * n], blk1, tmp[:, c, :], True, True, r=["kc", "tmp"], w=pk)
            S.tt(bon, p[:, 0:8 * n].rearrange("p (c t) -> p c t", c=8), vb, ALU.mult, r=pk + ["vb"], w=["bon"])
            S.ts(wl, wl, -0.6065306597126334, ALU.mult, r=["wl"], w=["wl"])
            for c in range(8):
                for q in range(2):
                    S.op("dve", lambda e: e.tensor_tensor_scan(out=wr(Eb[:, c, q * 64:(q + 1) * 64]), data0=ones[:, 0:64],
                                                               data1=wl[:, c, q * 64:(q + 1) * 64], initial=0.0,
                                                               op0=ALU.mult, op1=ALU.add), r=["wl", "kc"], w=["Eb"])
            S.act(tmp, Eb, AF.Exp, r=["Eb"], w=["tmp"], scale=-1.0)
            S.act(tmp2, Eb, AF.Exp, r=["Eb"], w=["tmp2"])
            S.tt(al, al, kk, ALU.mult, r=["al", "kk"], w=["al"])
            S.tt(al, al, tmp, ALU.mult, r=["al", "tmp"], w=["al"])
            S.tt(kb, kb, tmp, ALU.mult, r=["kb", "tmp"], w=["kb"])
            S.tt(rb, rb, tmp2, ALU.mult, r=["rb", "tmp2"], w=["rb"])
            wl4 = wl.rearrange("p c (q t) -> p c q t", q=2)
            E4 = tmp2.rearrange("p c (q t) -> p c q t", q=2)
            S.cp(wl4[:, :, :, 0:1], ones[:, 0:16].rearrange("p (c q o) -> p c q o", c=8, q=2), r=["wl", "kc"], w=["wl"])
            S.cp(wl4[:, :, :, 1:64], E4[:, :, :, 0:63], r=["tmp2", "wl"], w=["wl"])
            S.stt(kk, kk, -1.0, wl, ALU.mult, ALU.mult, r=["kk", "wl"], w=["kk"])
            wc = sm[:, 0:16].rearrange("p (c q) -> p c q", c=8)
            S.cp(wc, E4[:, :, :, 63], r=["tmp2"], w=["wc"])
            S.barrier()
            if qs == 0 and "prep" in DEBUG:
                for nm_, b_ in (("u", u), ("rb", rb), ("kb", kb), ("vb", vb), ("al", al), ("gb", gb), ("kk", kk), ("bon", bon), ("Eb", Eb), ("E", tmp2), ("Ep", wl)):
                    dbg(nm_, b_)

            freeb = [0, 1, 2, 6, 11, 13, 14]

            def tq(i):
                if i < 14:
                    off = freeb[i // 2] * nb + (i % 2) * 512
                else:
                    off = 15360 + (i - 14) * 512
                return U[:, off:off + 512].rearrange("p (c t) -> p c t", c=8)

            ARc = U[:, 0:1024].rearrange("p (c t) -> p c t", c=8)
            Bh, Kh, VT, BhT, KhT = tq(2), tq(3), tq(4), tq(5), tq(6)
            Mb, Pm, Nb, Qm, MTb = tq(7), tq(8), tq(9), tq(10), tq(11)
            Tms = [tq(12), tq(13)]
            Pb2, PTb2 = tq(14), tq(15)
            XT, UT, yc, sqv, tmpc = tq(16), tq(17), tq(18), tq(19), tq(20)
            s1 = sm[:, 16:24]
            s2 = sm[:, 24:32]
            heads = [(hh // 2, 64 * (hh % 2)) for hh in range(16)]
            idq = lambda p0: ident[p0:p0 + 64, p0:p0 + 64]
            v3 = lambda p_: p_[:, 0:512].rearrange("p (c t) -> p c t", c=8)

            def headmm(p_, lf, rf, start=True, stop=True, r=(), w=(), width=64):
                multi = start and not stop
                for (fc, p0) in heads:
                    st_ = (fc == 0) if multi else start
                    S.mm(p_[p0:p0 + 64, fc * width:(fc + 1) * width], lf(fc, p0), rf(fc, p0), st_, stop, r=list(r), w=list(w))

            for q in range(2):
                cs = slice(q * 64, (q + 1) * 64)
                S.act(ARc[:, :, 0:64], kk[:, :, cs], AF.Copy, r=["kk"], w=["ARc"])
                S.act(ARc[:, :, 64:128], rb[:, :, cs], AF.Copy, r=["rb"], w=["ARc"])
                wcb = bc(wc[:, :, q:q + 1], [128, 8, 64])
                S.tt(Bh, al[:, :, cs], wcb, ALU.mult, r=["al", "wc"], w=["Bh"])
                S.tt(Kh, kb[:, :, cs], wcb, ALU.mult, r=["kb", "wc"], w=["Kh"])
                p, pk = ps(1)
                headmm(p, lambda fc, p0: vb[p0:p0 + 64, fc, cs], lambda fc, p0: idq(p0), r=["vb", "kc"], w=pk)
                S.act(VT, v3(p), AF.Copy, r=pk, w=["VT"])
                p, pk = ps(1)
                headmm(p, lambda fc, p0: Bh[p0:p0 + 64, fc, :], lambda fc, p0: idq(p0), r=["Bh", "kc"], w=pk)
                S.cp(BhT, v3(p), r=pk, w=["BhT"])
                p, pk = ps(1)
                headmm(p, lambda fc, p0: Kh[p0:p0 + 64, fc, :], lambda fc, p0: idq(p0), r=["Kh", "kc"], w=pk)
                S.act(KhT, v3(p), AF.Copy, r=pk, w=["KhT"])
                p, pk = ps(2)
                headmm(p, lambda fc, p0: al[p0:p0 + 64, fc, cs], lambda fc, p0: ARc[p0:p0 + 64, fc, :], r=["al", "ARc"], w=pk, width=128)
                pv = p[:, 0:1024].rearrange("p (c t) -> p c t", c=8)
                S.tt(Mb, pv[:, :, 0:64], bc(tri_lt.unsqueeze(1), [128, 8, 64]), ALU.mult, r=pk + ["kc"], w=["Mb"])
                S.tt(Pm, pv[:, :, 64:128], bc(tri_le.unsqueeze(1), [128, 8, 64]), ALU.mult, r=pk + ["kc"], w=["Pm"])
                p, pk = ps(2)
                headmm(p, lambda fc, p0: kb[p0:p0 + 64, fc, cs], lambda fc, p0: ARc[p0:p0 + 64, fc, :], r=["kb", "ARc"], w=pk, width=128)
                pv = p[:, 0:1024].rearrange("p (c t) -> p c t", c=8)
                S.tt(Nb, pv[:, :, 0:64], bc(tri_lt.unsqueeze(1), [128, 8, 64]), ALU.mult, r=pk + ["kc"], w=["Nb"])
                S.tt(Qm, pv[:, :, 64:128], bc(tri_le.unsqueeze(1), [128, 8, 64]), ALU.mult, r=pk + ["kc"], w=["Qm"])
                p, pk = ps(1)
                headmm(p, lambda fc, p0: kk[p0:p0 + 64, fc, cs], lambda fc, p0: al[p0:p0 + 64, fc, cs], r=["kk", "al"], w=pk)
                S.tt(MTb, v3(p), bc(tri_gt.unsqueeze(1), [128, 8, 64]), ALU.mult, r=pk + ["kc"], w=["MTb"])
                S.tt(Tms[0], Mb, bc(i64.unsqueeze(1), [128, 8, 64]), ALU.add, r=["Mb", "kc"], w=["T0"])
                Pc, PTc, Pck, PTck = Mb, MTb, "Mb", "MTb"
                Pn, PTn, Pnk, PTnk = Pb2, PTb2, "Pb2", "PTb2"
                ti = 0
                for it in range(5):
                    p, pk = ps(1)
                    headmm(p, lambda fc, p0: PTc[p0:p0 + 64, fc, :], lambda fc, p0: Pc[p0:p0 + 64, fc, :], r=[Pck, PTck], w=pk)
                    p2, pk2 = ps(1)
                    headmm(p2, lambda fc, p0: Pc[p0:p0 + 64, fc, :], lambda fc, p0: PTc[p0:p0 + 64, fc, :], r=[Pck, PTck], w=pk2)
                    S.act(Pn, v3(p), AF.Copy, r=pk, w=[Pnk])
                    S.cp(PTn, v3(p2), r=pk2, w=[PTnk])
                    p3, pk3 = ps(1)
                    Tc, Tck = Tms[ti], f"T{ti}"
                    Tn, Tnk = Tms[1 - ti], f"T{1 - ti}"
                    headmm(p3, lambda fc, p0: PTn[p0:p0 + 64, fc, :], lambda fc, p0: Tc[p0:p0 + 64, fc, :], r=[PTnk, Tck], w=pk3)
                    S.tt(Tn, Tc, v3(p3), ALU.add, r=pk3 + [Tck], w=[Tnk])
                    ti = 1 - ti
                    Pc, PTc, Pck, PTck, Pn, PTn, Pnk, PTnk = Pn, PTn, Pnk, PTnk, Pc, PTc, Pck, PTck
                Tf, Tfk = Tms[ti], f"T{ti}"
                p, pk = ps(1)
                headmm(p, lambda fc, p0: kk[p0:p0 + 64, fc, cs], lambda fc, p0: Sr[j][p0:p0 + 64, fc, :], True, False, r=["kk", srk], w=pk)
                headmm(p, lambda fc, p0: Nb[p0:p0 + 64, fc, :], lambda fc, p0: VT[p0:p0 + 64, fc, :], False, True, r=["Nb", "VT"], w=pk)
                S.act(XT, v3(p), AF.Copy, r=pk, w=["XT"])
                p, pk = ps(1)
                headmm(p, lambda fc, p0: Tf[p0:p0 + 64, fc, :], lambda fc, p0: XT[p0:p0 + 64, fc, :], r=[Tfk, "XT"], w=pk)
                S.cp(UT, v3(p), r=pk, w=["UT"])
                py, pyk = ps(1)
                headmm(py, lambda fc, p0: rb[p0:p0 + 64, fc, cs], lambda fc, p0: Sr[j][p0:p0 + 64, fc, :], True, False, r=["rb", srk], w=pyk)
                headmm(py, lambda fc, p0: Pm[p0:p0 + 64, fc, :], lambda fc, p0: UT[p0:p0 + 64, fc, :], False, False, r=["Pm", "UT"], w=pyk)
                headmm(py, lambda fc, p0: Qm[p0:p0 + 64, fc, :], lambda fc, p0: VT[p0:p0 + 64, fc, :], False, True, r=["Qm", "VT"], w=pyk)
                p, pk = ps(1)
                headmm(p, lambda fc, p0: BhT[p0:p0 + 64, fc, :], lambda fc, p0: UT[p0:p0 + 64, fc, :], True, False, r=["BhT", "UT"], w=pk)
                headmm(p, lambda fc, p0: KhT[p0:p0 + 64, fc, :], lambda fc, p0: VT[p0:p0 + 64, fc, :], False, True, r=["KhT", "VT"], w=pk)
                S.tt(Sr[j][:, :, :], Sr[j][:, :, :], wcb, ALU.mult, r=[srk, "wc"], w=[srk])
                S.tt(Sr[j][:, :, :], Sr[j][:, :, :], v3(p), ALU.add, r=pk + [srk], w=[srk])
                S.act(yc, v3(py), AF.Copy, r=pyk, w=["yc"])
                p, pk = ps(1)
                headmm(p, lambda fc, p0: yc[p0:p0 + 64, fc, :], lambda fc, p0: idq(p0), r=["yc", "kc"], w=pk)
                yf = tq(21)
                S.cp(yf, v3(p), r=pk, w=["yf"])
                p, pk = ps(1)
                for fc in range(8):
                    S.mm(p[:, fc * 64:(fc + 1) * 64], blk1, yf[:, fc, :], True, True, r=["kc", "yf"], w=pk)
                S.stt(yf, v3(p), -1.0 / 64.0, yf, ALU.mult, ALU.add, r=pk + ["yf"], w=["yf"])
                S.tt(sqv, yf, yf, ALU.mult, r=["yf"], w=["sqv"])
                p, pk = ps(1)
                for fc in range(8):
                    S.mm(p[:, fc * 64:(fc + 1) * 64], blk1, sqv[:, fc, :], True, True, r=["kc", "sqv"], w=pk)
                S.ts(sqv, v3(p), 1.0 / 64.0, ALU.mult, r=pk, w=["sqv"], s2=64e-5, op1=ALU.add)
                S.act(sqv, sqv, AF.Sqrt, r=["sqv"], w=["sqv"])
                S.op("dve", lambda e: e.reciprocal(out=wr(sqv), in_=sqv), r=["sqv"], w=["sqv"])
                S.tt(yf, yf, sqv, ALU.mult, r=["yf", "sqv"], w=["yf"])
                S.tt(tmpc, yf, bc(cpc(f"r_lnx_w{j}").unsqueeze(2), [128, 8, 64]), ALU.mult, r=["yf", "cpt"], w=["tmpc"])
                S.tt(tmpc, tmpc, bc(cpc(f"r_lnx_b{j}").unsqueeze(2), [128, 8, 64]), ALU.add, r=["tmpc", "cpt"], w=["tmpc"])
                S.tt(tmpc, tmpc, bon[:, :, cs], ALU.add, r=["tmpc", "bon"], w=["tmpc"])
                S.tt(yg[:, :, cs], tmpc, gb[:, :, cs], ALU.mult, r=["tmpc", "gb"], w=["yg"])
                S.barrier()
                if qs == 0 and q == 0 and "chunk" in DEBUG:
                    for nm_, b_ in (("yf", yf), ("VT", VT), ("BhT", BhT), ("Mb", Mb), ("Pm", Pm), ("Nb", Nb), ("Qm", Qm), ("Tf", Tf), ("XT", XT), ("UT", UT), ("yc", yc), ("tmpc", tmpc), ("Sr", Sr[j][:, :, :])):
                        dbg(nm_, b_)
            S.barrier()
            proj(3, yg, "yg", xn[:, :, qs:qs + n], "xn")

        def state_list():
            lst = []
            for l in layers:
                jj = l // 2
                if l % 2 == 0:
                    lst.append((f"Sm{jj}", Sm[jj][:, :], jj * 2048, 2048, None))
                    lst.append((f"Cc{jj}", Cc[jj][:, :, :], 4096 + jj * 96, 96, "p (c k) -> p c k"))
                else:
                    lst.append((f"Sr{jj}", Sr[jj][:, :, :], 4288 + jj * 512, 512, "p (c k) -> p c k"))
                    lst.append((f"Up{jj}", Up[jj][:, :, :], 5312 + jj * 8, 8, "p (c k) -> p c k"))
            return lst

        sto_keys = []
        for s in range(NSEQ):
            for (key, tile_ap, off, n_, rr) in state_list():
                src = st_in[s, :, off:off + n_]
                if rr is not None:
                    src = src.rearrange(rr, c=tile_ap.shape[1])
                S.dma("pool", tile_ap, src, w=[key], stream="si" + key)
            for t0 in range(0, L, T):
                S.barrier()
                S.dma("pool", h[:, :, :], xT[s, :, :, t0:t0 + T], w=["h"], stream="xin")
                for l in layers:
                    S.barrier()
                    ffn(l, 0)
                    S.barrier()
                    gname = f"n{l}_2"
                    if l % 2 == 0:
                        prenorm(gname, 0, T)
                        for hs in range(0, T, TMM):
                            S.barrier()
                            mamba(l, hs)
                    else:
                        prenorm(gname, 0, T)
                        for qs in range(0, T, TMR):
                            S.barrier()
                            rwkv(l, qs)
                    S.barrier()
                    postnorm_add(f"n{l}_3", 1.0)
                    S.barrier()
                    ffn(l, 1)
                S.barrier()
                S.dma("pool", outT[s, :, :, t0:t0 + T], h[:, :, :], r=["h"], w=["OUT"], stream="xout")
            S.barrier()
            for (key, tile_ap, off, n_, rr) in state_list():
                dst = st_out[s, :, off:off + n_]
                if rr is not None:
                    dst = dst.rearrange(rr, c=tile_ap.shape[1])
                S.dma("pool", dst, tile_ap, r=[key], w=["STO" + key + str(s)], stream="so" + key)
                sto_keys.append("STO" + key + str(s))
        S.wait_keys("pool", ["OUT"] + sto_keys)
    return nc


def run_layers(inputs, x, layers, n_cores, lseg=None):
    Bn, L, _ = x.shape
    NSEQ = Bn // n_cores
    lseg = lseg or L
    cp = pack_consts(inputs, layers)
    cparr = cp.get()
    W = pack_weights(inputs, layers)
    wshapes = {k: v.shape for k, v in W.items()}
    nc = build(NSEQ, lseg, layers, cp.off, cparr.shape[1], wshapes)
    kc = const_mats()
    ones = np.ones((128, 128), np.float32)
    xT = np.asarray(x, np.float32).reshape(Bn, L, 8, 128).transpose(0, 3, 2, 1)
    global LAST_DBG
    states = [np.zeros((NSEQ, 128, NST), np.float32) for _ in range(n_cores)]
    oT = np.empty((Bn, 128, 8, L), np.float32)
    for g in range(L // lseg):
        in_maps = []
        for c in range(n_cores):
            m = {"xT": np.ascontiguousarray(xT[c * NSEQ:(c + 1) * NSEQ, :, :, g * lseg:(g + 1) * lseg]),
                 "st_in": states[c], "cp": cparr, "kc": kc, "ones_r": ones}
            m.update(W)
            in_maps.append(m)
        res = run_bass_kernel_spmd(nc, in_maps, core_ids=list(range(n_cores)))
        LAST_DBG = {k: np.asarray(v) for k, v in res.results[0].items() if k.startswith("dbg_")}
        for c in range(n_cores):
            oT[c * NSEQ:(c + 1) * NSEQ, :, :, g * lseg:(g + 1) * lseg] = np.asarray(res.results[c]["outT"])
            states[c] = np.ascontiguousarray(np.asarray(res.results[c]["st_out"], np.float32))
    return np.ascontiguousarray(oT.transpose(0, 3, 2, 1)).reshape(Bn, L, D)


LSEG = 1024


def kernel(**inputs):
    x = np.asarray(inputs["x"], np.float32)
    return run_layers(inputs, x, [0, 1, 2, 3], 8, LSEG)
```
